# Optimizing a Trainium2 kernel written in Bass

```python
import math
import jax, jax.numpy as jnp
from jax import lax
import numpy as np

D_MODEL = 2048
BATCH = 4
SEQ = 2048
DEPTH = 1

GRID_W = 64
D_SSM = 1024
SSM_GROUP = 16
N_SSM_GROUPS = D_SSM // SSM_GROUP
SSM_STATE = 64
DT_MIN = 1e-3
DT_MAX = 1e-1
N_HEADS = 16
HEAD_DIM = 64
D_ATTN = N_HEADS * HEAD_DIM
WIN_H_MAX = 8
WIN_W = 16
D_MIX = D_SSM + D_ATTN
D_IN_PROJ = D_SSM + 3 * D_ATTN
N_EXPERTS = 16
CAPACITY_FACTOR = 2
D_FF_EXPERT = 2816
EPS = 1e-6

kernel_name = "hybrid_s5_natten_ecmoe_encoder"


def rms_norm(x, g):
    xf = x.astype(jnp.float32)
    y = xf * lax.rsqrt(jnp.mean(xf * xf, axis=-1, keepdims=True) + EPS)
    return (y * g.astype(jnp.float32)).astype(x.dtype)


def modulate(h, shift, scale):
    return h * (1.0 + scale[:, None, :]) + shift[:, None, :]


def _complex_linear_scan(a_re, a_im, b_re, b_im):
    def combine(e1, e2):
        a1r, a1i, b1r, b1i = e1
        a2r, a2i, b2r, b2i = e2
        ar = a1r * a2r - a1i * a2i
        ai = a1r * a2i + a1i * a2r
        br = a2r * b1r - a2i * b1i + b2r
        bi = a2r * b1i + a2i * b1r + b2i
        return (ar, ai, br, bi)
    _, _, xr, xi = lax.associative_scan(combine, (a_re, a_im, b_re, b_im), axis=1)
    return xr, xi


def s5_mixer(u, a_re, a_im, log_dt, b_re, b_im, c_re, c_im, d_skip, w_glu, b_glu):
    bsz, L, _ = u.shape
    ug = u.reshape(bsz, L, N_SSM_GROUPS, SSM_GROUP).astype(jnp.float32)
    y = (d_skip * u).astype(jnp.float32).reshape(bsz, L, N_SSM_GROUPS, SSM_GROUP)
    for direction in range(2):
        dt = jnp.exp(log_dt[direction].astype(jnp.float32))[:, None]
        lr = a_re[direction].astype(jnp.float32)
        li = a_im[direction].astype(jnp.float32)
        mag = jnp.exp(lr * dt)
        lbr = mag * jnp.cos(li * dt)
        lbi = mag * jnp.sin(li * dt)
        den = lr * lr + li * li
        nr = lbr - 1.0
        fr = (nr * lr + lbi * li) / den
        fi = (lbi * lr - nr * li) / den
        br = b_re[direction].astype(jnp.float32)
        bi = b_im[direction].astype(jnp.float32)
        bbr = fr[..., None] * br - fi[..., None] * bi
        bbi = fr[..., None] * bi + fi[..., None] * br
        bu_re = jnp.einsum('blgh,gph->blgp', ug, bbr)
        bu_im = jnp.einsum('blgh,gph->blgp', ug, bbi)
        if direction == 1:
            bu_re = jnp.flip(bu_re, axis=1)
            bu_im = jnp.flip(bu_im, axis=1)
        ar = jnp.broadcast_to(lbr[None, None], (1, L, N_SSM_GROUPS, SSM_STATE))
        ai = jnp.broadcast_to(lbi[None, None], (1, L, N_SSM_GROUPS, SSM_STATE))
        xr, xi = _complex_linear_scan(ar, ai, bu_re, bu_im)
        if direction == 1:
            xr = jnp.flip(xr, axis=1)
            xi = jnp.flip(xi, axis=1)
        y = y + jnp.einsum('blgp,ghp->blgh', xr, c_re[direction].astype(jnp.float32)) \
              - jnp.einsum('blgp,ghp->blgh', xi, c_im[direction].astype(jnp.float32))
    y = y.reshape(bsz, L, D_SSM).astype(u.dtype)
    z = jax.nn.gelu(y)
    return z * jax.nn.sigmoid(z @ w_glu + b_glu)


def neighbourhood_attention(q, k, v, rpb):
    bsz, L, H, Dh = q.shape
    rows = L // GRID_W
    kh = min(WIN_H_MAX, rows)
    r = jnp.arange(rows)
    row_start = jnp.clip(r - kh // 2, 0, rows - kh)
    row_idx = row_start[:, None] + jnp.arange(kh)[None, :]
    w = jnp.arange(GRID_W)
    col_start = jnp.clip(w - WIN_W // 2, 0, GRID_W - WIN_W)
    col_mask = (w[None, :] >= col_start[:, None]) & (w[None, :] < col_start[:, None] + WIN_W)

    qg = q.reshape(bsz, rows, GRID_W, H, Dh)
    kg = k.reshape(bsz, rows, GRID_W, H, Dh)[:, row_idx]
    vg = v.reshape(bsz, rows, GRID_W, H, Dh)[:, row_idx]
    s = jnp.einsum('brqhd,brjkhd->bhrqjk', qg, kg).astype(jnp.float32) * (Dh ** -0.5)
    dr = row_idx - r[:, None] + (WIN_H_MAX - 1)
    dc = jnp.clip(w[None, :] - w[:, None] + (WIN_W - 1), 0, 2 * WIN_W - 2)
    bias = rpb[:, dr[:, None, :, None], dc[None, :, None, :]].astype(jnp.float32)
    s = s + bias[None]
    s = jnp.where(col_mask[None, None, None, :, None, :], s, jnp.float32(-1e30))
    shp = s.shape
    p = jax.nn.softmax(s.reshape(shp[:-2] + (kh * GRID_W,)), axis=-1).reshape(shp)
    out = jnp.einsum('bhrqjk,brjkhd->brqhd', p.astype(v.dtype), vg)
    return out.reshape(bsz, L, H * Dh)


def expert_choice_ffn(h, w_router, w_gate, w_up, w_down):
    bsz, L, _ = h.shape
    cap = CAPACITY_FACTOR * L // N_EXPERTS
    logits = jnp.einsum('bld,de->ble', h, w_router).astype(jnp.float32)
    aff = jax.nn.softmax(logits, axis=-1)
    top_aff, top_idx = lax.top_k(jnp.swapaxes(aff, 1, 2), cap)
    b_idx = jnp.arange(bsz)[:, None, None]
    xe = h[b_idx, top_idx]
    hid = jax.nn.silu(jnp.einsum('becd,edf->becf', xe, w_gate)) * jnp.einsum('becd,edf->becf', xe, w_up)
    ye = jnp.einsum('becf,efd->becd', hid, w_down) * top_aff[..., None].astype(h.dtype)
    return jnp.zeros_like(h).at[b_idx, top_idx].add(ye)


def setup_inputs(seed: int = 0) -> dict:
    key = jax.random.key(seed)
    ks = jax.random.split(key, 32)
    f32 = jnp.float32

    def nrm(k, shape, s):
        return jax.random.normal(k, shape, f32) * s

    G, P, H = N_SSM_GROUPS, SSM_STATE, SSM_GROUP
    n = jnp.arange(P, dtype=f32)
    return {
        "x": nrm(ks[0], (BATCH, SEQ, D_MODEL), 1.0),
        "c": nrm(ks[1], (BATCH, D_MODEL), 1.0),
        "w_ada": nrm(ks[2], (DEPTH, D_MODEL, 6 * D_MODEL), 0.5 * D_MODEL ** -0.5),
        "b_ada": nrm(ks[3], (DEPTH, 6 * D_MODEL), 0.02),
        "g_mix": 1.0 + nrm(ks[4], (DEPTH, D_MODEL), 0.02),
        "w_in": nrm(ks[5], (DEPTH, D_MODEL, D_IN_PROJ), D_MODEL ** -0.5),
        "ssm_a_re": -0.5 + nrm(ks[6], (DEPTH, 2, G, P), 0.02),
        "ssm_a_im": math.pi * n + nrm(ks[7], (DEPTH, 2, G, P), 0.02),
        "ssm_log_dt": jax.random.uniform(ks[8], (DEPTH, 2, G), f32, math.log(DT_MIN), math.log(DT_MAX)),
        "ssm_b_re": nrm(ks[9], (DEPTH, 2, G, P, H), (2 * H) ** -0.5),
        "ssm_b_im": nrm(ks[10], (DEPTH, 2, G, P, H), (2 * H) ** -0.5),
        "ssm_c_re": nrm(ks[11], (DEPTH, 2, G, H, P), (2 * P) ** -0.5),
        "ssm_c_im": nrm(ks[12], (DEPTH, 2, G, H, P), (2 * P) ** -0.5),
        "ssm_d": nrm(ks[13], (DEPTH, D_SSM), 1.0),
        "w_glu": nrm(ks[14], (DEPTH, D_SSM, D_SSM), D_SSM ** -0.5),
        "b_glu": nrm(ks[15], (DEPTH, D_SSM), 0.02),
        "rpb": nrm(ks[16], (DEPTH, N_HEADS, 2 * WIN_H_MAX - 1, 2 * WIN_W - 1), 0.1),
        "g_ssm_out": 1.0 + nrm(ks[17], (DEPTH, D_SSM), 0.02),
        "g_attn_out": 1.0 + nrm(ks[18], (DEPTH, D_ATTN), 0.02),
        "w_out": nrm(ks[19], (DEPTH, D_MIX, D_MODEL), D_MIX ** -0.5),
        "g_ffn": 1.0 + nrm(ks[20], (DEPTH, D_MODEL), 0.02),
        "w_router": nrm(ks[21], (DEPTH, D_MODEL, N_EXPERTS), D_MODEL ** -0.5),
        "w_gate": nrm(ks[22], (DEPTH, N_EXPERTS, D_MODEL, D_FF_EXPERT), D_MODEL ** -0.5),
        "w_up": nrm(ks[23], (DEPTH, N_EXPERTS, D_MODEL, D_FF_EXPERT), D_MODEL ** -0.5),
        "w_down": nrm(ks[24], (DEPTH, N_EXPERTS, D_FF_EXPERT, D_MODEL), D_FF_EXPERT ** -0.5),
        "g_final": 1.0 + nrm(ks[25], (D_MODEL,), 0.02),
    }


def reference(x, c, w_ada, b_ada, g_mix, w_in, ssm_a_re, ssm_a_im, ssm_log_dt,
              ssm_b_re, ssm_b_im, ssm_c_re, ssm_c_im, ssm_d, w_glu, b_glu, rpb,
              g_ssm_out, g_attn_out, w_out, g_ffn, w_router, w_gate, w_up, w_down,
              g_final):
    bsz, L, _ = x.shape
    c_act = jax.nn.silu(c)
    for layer in range(DEPTH):
        mod = c_act @ w_ada[layer] + b_ada[layer]
        sh1, sc1, gt1, sh2, sc2, gt2 = jnp.split(mod, 6, axis=-1)

        h = modulate(rms_norm(x, g_mix[layer]), sh1, sc1)
        proj = h @ w_in[layer]
        u, q, k, v = jnp.split(proj, [D_SSM, D_SSM + D_ATTN, D_SSM + 2 * D_ATTN], axis=-1)
        y_ssm = s5_mixer(u, ssm_a_re[layer], ssm_a_im[layer], ssm_log_dt[layer],
                         ssm_b_re[layer], ssm_b_im[layer], ssm_c_re[layer], ssm_c_im[layer],
                         ssm_d[layer], w_glu[layer], b_glu[layer])
        y_attn = neighbourhood_attention(q.reshape(bsz, L, N_HEADS, HEAD_DIM),
                                         k.reshape(bsz, L, N_HEADS, HEAD_DIM),
                                         v.reshape(bsz, L, N_HEADS, HEAD_DIM), rpb[layer])
        mixed = jnp.concatenate([rms_norm(y_ssm, g_ssm_out[layer]),
                                 rms_norm(y_attn, g_attn_out[layer])], axis=-1)
        x = x + gt1[:, None, :] * (mixed @ w_out[layer])

        h = modulate(rms_norm(x, g_ffn[layer]), sh2, sc2)
        x = x + gt2[:, None, :] * expert_choice_ffn(h, w_router[layer], w_gate[layer],
                                                     w_up[layer], w_down[layer])
    return rms_norm(x, g_final)
```

```python
import contextlib
import numpy as np
import concourse.bass as bass
import concourse.mybir as mybir
from concourse.bass_utils import run_bass_kernel_spmd

F32 = mybir.dt.float32
BF16 = mybir.dt.bfloat16
AF = mybir.ActivationFunctionType
ALU = mybir.AluOpType

SAME_ENGINE_SYNC = True
NS_DMA = 8
D = 2048
L = 2048
EPS = 1e-6


class Node:
    __slots__ = ("eng", "fn", "deps", "sig", "sem", "val", "is_dma", "idx", "pre")

    def __init__(self, eng, fn, is_dma):
        self.eng = eng
        self.fn = fn
        self.is_dma = is_dma
        self.deps = ()
        self.sig = is_dma
        self.sem = None
        self.val = 0
        self.pre = None


class Prog:
    ENGS = ["pe", "dve", "act", "pool", "sp"]

    def __init__(self):
        self.nc = nc = bass.Bass("TRN2", target_bir_lowering=False)
        self.pstack = contextlib.ExitStack()
        self.stack = None
        self.esem = {e: self.pstack.enter_context(nc.semaphore("s_" + e)) for e in self.ENGS}
        self.dsem = {
            e: [self.pstack.enter_context(nc.semaphore("d_%s%d" % (e, i))) for i in range(NS_DMA)]
            for e in ("sp", "pool", "act")
        }
        self.cnt = {e: 0 for e in self.ENGS}
        self.dcnt = {e: 0 for e in self.ENGS}
        self.tokens = []
        self.out_dmas = []
        self._reset()

    def _reset(self):
        self.nodes = []
        self.state = {}
        self.byroot = {}

    def psb(self, name, shape, dtype):
        return self.pstack.enter_context(self.nc.sbuf_tensor(name + "_pp", list(shape), dtype))

    def sb(self, name, shape, dtype):
        return self.stack.enter_context(self.nc.sbuf_tensor("%s_p%d" % (name, self.phase), list(shape), dtype))

    def ps(self, name, shape, dtype=F32):
        return self.stack.enter_context(self.nc.psum_tensor("%s_p%d" % (name, self.phase), list(shape), dtype))

    def begin(self):
        self.phase = getattr(self, "phase", 0) + 1
        self.stack = contextlib.ExitStack()

    def _related(self, k):
        out = []
        for kk in self.byroot.get(k[0], ()):
            n = min(len(kk), len(k))
            if kk[:n] == k[:n]:
                out.append(kk)
        return out

    def add(self, eng, fn, r=(), w=(), dma=False):
        node = Node(eng, fn, dma)
        node.idx = len(self.nodes)
        deps = set()
        r = [k if isinstance(k, tuple) else (k,) for k in r]
        w = [k if isinstance(k, tuple) else (k,) for k in w]
        for k in r:
            for kk in self._related(k):
                lw = self.state[kk][0]
                if lw is not None:
                    deps.add(lw)
        for k in w:
            for kk in self._related(k):
                lw, rd = self.state[kk]
                if lw is not None:
                    deps.add(lw)
                deps.update(rd)
        for k in r:
            st = self.state.get(k)
            if st is None:
                self.state[k] = [None, [node]]
                self.byroot.setdefault(k[0], []).append(k)
            else:
                rd = st[1]
                if not dma:
                    rd = [x for x in rd if x.is_dma or x.eng != eng]
                rd.append(node)
                st[1] = rd
        for k in w:
            for kk in self._related(k):
                if len(kk) > len(k):
                    del self.state[kk]
                    self.byroot[k[0]].remove(kk)
            st = self.state.get(k)
            if st is None:
                self.state[k] = [node, []]
                self.byroot.setdefault(k[0], []).append(k)
            else:
                st[0] = node
                st[1] = []
        deps.discard(node)
        node.deps = deps
        self.nodes.append(node)
        return node

    def dma(self, q, out, in_, r=(), w=()):
        return self.add(q, lambda e: e.dma_start(out=out, in_=in_), r, w, dma=True)

    def mm(self, out, lhsT, rhs, start, stop, r, w):
        return self.add("pe", lambda e: e.matmul(out, lhsT=lhsT, rhs=rhs, start=start, stop=stop), r, w)

    def tr(self, out, in_, ident, r, w):
        return self.add("pe", lambda e: e.transpose(out=out, in_=in_, identity=ident), r, w)

    def act(self, out, in_, func, r, w, **kw):
        return self.add("act", lambda e: e.activation(out=out, in_=in_, func=func, **kw), r, w)

    def ts(self, eng, out, in0, s1, s2, op0, op1, r, w):
        if op1 is None:
            return self.add(eng, lambda e: e.tensor_scalar(out=out, in0=in0, scalar1=s1, scalar2=None, op0=op0), r, w)
        return self.add(eng, lambda e: e.tensor_scalar(out=out, in0=in0, scalar1=s1, scalar2=s2, op0=op0, op1=op1), r, w)

    def tt(self, eng, out, in0, in1, op, r, w):
        return self.add(eng, lambda e: e.tensor_tensor(out=out, in0=in0, in1=in1, op=op), r, w)

    def stt(self, eng, out, in0, scalar, in1, op0, op1, r, w):
        return self.add(eng, lambda e: e.scalar_tensor_tensor(out=out, in0=in0, scalar=scalar, in1=in1, op0=op0, op1=op1), r, w)

    def cp(self, eng, out, in_, r, w):
        if eng == "act":
            return self.add(eng, lambda e: e.copy(out=out, in_=in_), r, w)
        return self.add(eng, lambda e: e.tensor_copy(out=out, in_=in_), r, w)

    def memset(self, eng, out, val, w):
        return self.add(eng, lambda e: e.memset(out, val), (), w)

    def end(self, final=False):
        nc = self.nc
        nodes = self.nodes
        for n in nodes:
            best = {}
            dl = []
            for d in n.deps:
                if d.is_dma:
                    dl.append(d)
                else:
                    if d.eng == n.eng and not n.is_dma and (n.eng == "pe" or not SAME_ENGINE_SYNC):
                        continue
                    b = best.get(d.eng)
                    if b is None or d.idx > b.idx:
                        best[d.eng] = d
            for d in best.values():
                d.sig = True
                dl.append(d)
            n.deps = dl
        per = {e: [n for n in nodes if n.eng == e] for e in self.ENGS}
        for e in self.ENGS:
            comp = [n for n in per[e] if not n.is_dma]
            if comp:
                comp[-1].sig = True
        for n in nodes:
            if n.is_dma:
                j = self.dcnt[n.eng]
                self.dcnt[n.eng] += 1
                n.sem = self.dsem[n.eng][j % NS_DMA]
                n.val = 16 * (j // NS_DMA + 1)
                n.pre = (n.sem, n.val - 16) if j >= NS_DMA else None
            elif n.sig:
                self.cnt[n.eng] += 1
                n.sem = self.esem[n.eng]
                n.val = self.cnt[n.eng]
        prev_tokens = self.tokens
        out_dmas = self.out_dmas

        def run(e, lst, last=False):
            waited = {}

            def wait(sem, val):
                key = id(sem)
                if waited.get(key, 0) >= val:
                    return
                waited[key] = val
                e.wait_ge(sem, val)

            if lst or last:
                for (sem, val) in prev_tokens:
                    wait(sem, val)
            for n in lst:
                if n.pre is not None:
                    wait(*n.pre)
                for d in n.deps:
                    wait(d.sem, d.val)
                ins = n.fn(e)
                if n.sig:
                    ins.then_inc(n.sem, 16 if n.is_dma else 1)
            if last:
                for n in out_dmas:
                    wait(n.sem, n.val)

        with nc.Block() as block:
            block.tensor(lambda e: run(e, per["pe"]))
            block.vector(lambda e: run(e, per["dve"]))
            block.scalar(lambda e: run(e, per["act"]))
            block.gpsimd(lambda e: run(e, per["pool"]))
            block.sync(lambda e: run(e, per["sp"], final))
        toks = {}
        for (sem, val) in prev_tokens:
            toks[id(sem)] = (sem, val)
        for n in nodes:
            if n.sem is not None:
                t = toks.get(id(n.sem))
                if t is None or t[1] < n.val:
                    toks[id(n.sem)] = (n.sem, n.val)
        self.tokens = list(toks.values())
        self.stack.close()
        self.stack = None
        self._reset()
        return nc


def build(dbg=None):
    P = Prog()
    nc = P.nc

    def din(name, shape, dt=F32):
        return nc.dram_tensor(name, list(shape), dt, kind="ExternalInput").ap()

    def dscratch(name, shape, dt):
        return nc.dram_tensor(name, list(shape), dt).ap()

    def dbg_dump(name, src, shape, dt, r):
        o = nc.dram_tensor("dbg_" + name, list(shape), dt, kind="ExternalOutput").ap()
        P.out_dmas.append(P.dma("sp", o, src, r=r))

    x = din("x", [L, D])
    cT = din("cT", [128, 16])
    w_ada = din("w_ada", [D, 6 * D])
    badaT = din("badaT", [128, 96])
    bada = din("bada", [1, 6 * D])
    gmixT = din("gmixT", [128, 16])
    w_in = din("w_in", [D, 4096])
    identd = din("identd", [128, 128])
    gfin = din("gfin", [128, D])
    jswd = din("jswd", [128, 128])
    ssm_lr = din("ssm_lr", [128, 128])
    ssm_li = din("ssm_li", [128, 128])
    ssm_ldt = din("ssm_ldt", [128, 128])
    cvec = din("cvec", [128, 4])
    Bp = din("Bp", [8, 2, 128, 8, 128])
    Crp = din("Crp", [8, 128, 16, 128])
    Cip = din("Cip", [8, 128, 16, 128])
    dcol_d = din("dcol", [128, 8])
    w_glu = din("w_glu", [1024, 1024])
    colv = din("colv", [128, 3, 8])
    biasE = din("biasE", [8, 128, 2, 14, 64])
    amask_d = din("amask", [128, 64])
    onesP_d = din("onesP", [128, 2, 128])
    hmask_d = din("hmask", [128, 2])
    w_out = din("w_out", [D, D])
    gffnT = din("gffnT", [128, 16])
    wr_d = din("wr", [128, 16, 16])
    sel_d = din("sel", [128, 2])
    need_moe = dbg is None or dbg in ("p6s",)
    if need_moe:
        w_gate = din("w_gate", [16, D, 2816])
        w_up = din("w_up", [16, D, 2816])
        w_down = din("w_down", [16, 2816, D])
    out = nc.dram_tensor("out", [1024, D], F32, kind="ExternalOutput").ap()
    xmid_d = dscratch("xmid_d", [L, D], F32)
    h2all_d = dscratch("h2all_d", [128, 16, L], BF16)

    fm_d = dscratch("fm_d", [24, 128, L], BF16)
    v_d = dscratch("v_d", [L, 1024], BF16)

    identb = P.psb("identb", [128, 128], BF16)
    ones1 = P.psb("ones1", [1, 128], F32)
    modT = P.psb("modT", [128, 96], F32)
    gt1b = P.psb("gt1b", [128, D], F32)
    gt2b = P.psb("gt2b", [128, D], F32)
    rstd = P.psb("rstd", [128, 2, 16], F32)
    colv_t = P.psb("colv_t", [128, 3, 8], F32)
    onesc = P.psb("onesc", [128, 1], BF16)
    aff_all = P.psb("aff_all", [128, 16, 16], F32)
    sel = P.psb("sel", [128, 2], F32)
    If32p = P.psb("If32p", [128, 128], F32)

    w_ada_v = w_ada.rearrange("(kt p) n -> p kt n", p=128)
    w_in_v = w_in.rearrange("(kt p) n -> p kt n", p=128)

    P.begin()
    cact = P.sb("cact", [128, 16], BF16)
    cst = P.sb("cst", [128, 16], F32)
    badaT_t = P.sb("badaT_t", [128, 96], F32)
    brow = P.sb("brow", [1, 512], F32)
    gmix_t = P.sb("gmix_t", [128, 16], F32)
    G1 = P.sb("G1", [128, 16], F32)
    rowt = P.sb("rowt", [1, 512], F32)
    wbuf = [P.sb("wbuf%d" % i, [128, 16, 512], BF16) for i in range(2)]
    pb = [P.ps("pb%d" % i, [128, 512], F32) for i in range(6)]
    pbt = [P.ps("pbt%d" % i, [128, 8, 128], BF16) for i in range(2)]
    hT = P.sb("hT", [128, 16, L], BF16)
    xt = [P.sb("xt%d" % i, [128, D], F32) for i in range(2)]
    junk = P.sb("junk", [128, D], BF16)
    xn = P.sb("xn", [128, D], BF16)
    ssq = P.sb("ssq", [128, 4], F32)
    stage = [P.sb("stage%d" % i, [128, L], BF16) for i in range(2)]
    vst = [P.sb("vst%d" % i, [128, 512], BF16) for i in range(2)]

    P.dma("pool", identb[:], identd, w=["identb"])
    P.dma("sp", cst[:], cT, w=["cst"])
    P.dma("sp", badaT_t[:], badaT, w=["badaT_t"])
    P.dma("sp", gmix_t[:], gmixT, w=["gmix_t"])
    P.memset("dve", ones1[:], 1.0, w=["ones1"])
    P.act(cact[:], cst[:], AF.Silu, r=["cst"], w=["cact"])

    wcount = [0]

    def load_w(src_view, c0):
        i = wcount[0] % 2
        wcount[0] += 1
        P.dma("pool", wbuf[i][:], src_view[:, :, c0:c0 + 512], w=[("wbuf", i)])
        return i

    pbc = [0]

    def nextpb():
        pbc[0] = (pbc[0] + 1) % 6
        return pbc[0]

    def ada_chunk(j, rowdst=None):
        i = load_w(w_ada_v, 512 * j)
        pi = nextpb()
        for m in range(4):
            for kt in range(16):
                P.mm(pb[pi][:, m:m + 1], lhsT=wbuf[i][:, kt, 128 * m:128 * m + 128], rhs=cact[:, kt:kt + 1],
                     start=(kt == 0), stop=(kt == 15), r=[("wbuf", i), "cact"], w=[("pb", pi)])
        P.tt("dve", modT[:, 4 * j:4 * j + 4], pb[pi][:, 0:4], badaT_t[:, 4 * j:4 * j + 4], ALU.add,
             r=[("pb", pi), "badaT_t"], w=[("modT", j)])
        if rowdst is not None:
            pi = nextpb()
            P.dma("sp", brow[:], bada[0:1, 512 * j:512 * j + 512], w=["brow"])
            for kt in range(16):
                P.mm(pb[pi][0:1, :], lhsT=cact[:, kt:kt + 1], rhs=wbuf[i][:, kt, :], start=(kt == 0), stop=(kt == 15),
                     r=[("wbuf", i), "cact"], w=[("pb", pi)])
            P.tt("dve", rowt[:], pb[pi][0:1, :], brow[:], ALU.add, r=[("pb", pi), "brow"], w=["rowt"])
            pi = nextpb()
            P.mm(pb[pi][:, :], lhsT=ones1[0:1, :], rhs=rowt[0:1, :], start=True, stop=True, r=["rowt", "ones1"], w=[("pb", pi)])
            P.cp("act", rowdst, pb[pi][:, :], r=[("pb", pi)], w=["gtb"])

    for j in range(8):
        ada_chunk(j)
    P.stt("dve", G1[:], modT[:, 16:32], 1.0, gmix_t[:], ALU.add, ALU.mult, r=["modT", "gmix_t"], w=["G1"])

    for tt_ in range(16):
        b = tt_ % 2
        P.dma("sp", xt[b][:], x[128 * tt_:128 * tt_ + 128, :], w=[("xt", b)])
        P.act(junk[:], xt[b][:], AF.Square, r=[("xt", b)], w=["junk", "ssq"], accum_out=ssq[:, 0:1])
        P.ts("dve", ssq[:, 1:2], ssq[:, 0:1], 1.0 / D, EPS, ALU.mult, ALU.add, r=["ssq"], w=["ssq"])
        P.act(ssq[:, 2:3], ssq[:, 1:2], AF.Sqrt, r=["ssq"], w=["ssq"])
        P.add("dve", lambda e: e.reciprocal(out=ssq[:, 3:4], in_=ssq[:, 2:3]), r=["ssq"], w=["ssq"])
        P.ts("dve", xn[:], xt[b][:], ssq[:, 3:4], None, ALU.mult, None, r=["ssq", ("xt", b)], w=["xn"])
        for half in range(2):
            for q in range(8):
                kt = 8 * half + q
                P.tr(pbt[half][:, q, :], xn[:, 128 * kt:128 * kt + 128], identb[:], r=["xn", "identb"], w=[("pbt", half)])
            for q in range(8):
                kt = 8 * half + q
                dst = hT[:, kt, 128 * tt_:128 * tt_ + 128]
                if half == 0:
                    P.act(dst, pbt[half][:, q, :], AF.Identity, r=[("pbt", half), "G1", "modT"], w=[("hT", tt_)],
                          scale=G1[:, kt:kt + 1], bias=modT[:, kt:kt + 1])
                else:
                    P.ts("dve", dst, pbt[half][:, q, :], G1[:, kt:kt + 1], modT[:, kt:kt + 1], ALU.mult, ALU.add,
                         r=[("pbt", half), "G1", "modT"], w=[("hT", tt_)])

    for j in range(8, 24):
        rowdst = None
        if 8 <= j < 12:
            rowdst = gt1b[:, 512 * (j - 8):512 * (j - 8) + 512]
        if 20 <= j < 24:
            rowdst = gt2b[:, 512 * (j - 20):512 * (j - 20) + 512]
        ada_chunk(j, rowdst)

    sc_ = [0]
    for jc in range(6):
        i = load_w(w_in_v, 512 * jc)
        for m in range(4):
            sb_i = sc_[0] % 2
            sc_[0] += 1
            for tc in range(4):
                pi = nextpb()
                for kt in range(16):
                    P.mm(pb[pi][:, :], lhsT=wbuf[i][:, kt, 128 * m:128 * m + 128], rhs=hT[:, kt, 512 * tc:512 * tc + 512],
                         start=(kt == 0), stop=(kt == 15), r=[("wbuf", i), "hT"], w=[("pb", pi)])
                P.cp("act" if tc % 2 else "dve", stage[sb_i][:, 512 * tc:512 * tc + 512], pb[pi][:, :],
                     r=[("pb", pi)], w=[("stage", sb_i, tc)])
            P.dma("sp", fm_d[4 * jc + m], stage[sb_i][:], r=[("stage", sb_i)], w=[("fm_d", 4 * jc + m)])
    for jc in range(6, 8):
        i = load_w(w_in_v, 512 * jc)
        for tt_ in range(16):
            pi = nextpb()
            vb = sc_[0] % 2
            sc_[0] += 1
            for kt in range(16):
                P.mm(pb[pi][:, :], lhsT=hT[:, kt, 128 * tt_:128 * tt_ + 128], rhs=wbuf[i][:, kt, :],
                     start=(kt == 0), stop=(kt == 15), r=[("wbuf", i), "hT"], w=[("pb", pi)])
            P.cp("act" if tt_ % 2 else "dve", vst[vb][:], pb[pi][:, :], r=[("pb", pi)], w=[("vst", vb)])
            P.dma("sp", v_d[128 * tt_:128 * tt_ + 128, 512 * (jc - 6):512 * (jc - 6) + 512], vst[vb][:],
                  r=[("vst", vb)], w=[("v_d", tt_, jc)])
    if dbg == "p1":
        dbg_dump("modT", modT[:], [128, 96], F32, ["modT"])
        dbg_dump("gt1b", gt1b[:], [128, D], F32, ["gtb"])
        dbg_dump("gt2b", gt2b[:], [128, D], F32, ["gtb"])
        P.end()
        P.begin()
        t0 = P.sb("t0", [128, L], BF16)
        for i in range(24):
            P.dma("sp", t0[:], fm_d[i], w=["t0"])
            o = nc.dram_tensor("dbg_fm%d" % i, [128, L], BF16, kind="ExternalOutput").ap()
            P.out_dmas.append(P.dma("sp", o, t0[:], r=["t0"]))
        t1 = P.sb("t1", [128, 16, 1024], BF16)
        P.dma("sp", t1[:], v_d.rearrange("(t p) c -> p t c", p=128), w=["t1"])
        dbg_dump("v", t1[:], [128, 16, 1024], BF16, ["t1"])
        return P.end(final=True)
    P.end()


    carry = contextlib.ExitStack()

    def csb(name, shape, dtype):
        return carry.enter_context(nc.sbuf_tensor(name + "_c", list(shape), dtype))

    zT = csb("zT", [128, 8, L], BF16)
    P.begin()
    SIG = [1, 2, 3, 4, 8, 12, 16, 32, 48, 64, 128, 192, 256, 512, 768, 1024]
    If32 = P.sb("If32", [128, 128], F32)
    Jf32 = P.sb("Jf32", [128, 128], F32)
    cv = P.sb("cv", [128, 4], F32)
    dcol = P.sb("dcol", [128, 8], F32)
    lr = P.sb("lr", [128, 128], F32)
    li = P.sb("li", [128, 128], F32)
    ldt = P.sb("ldt", [128, 128], F32)
    tmp = [P.sb("tmp%d" % i, [128, 128], F32) for i in range(8)]
    qi = P.sb("qi", [128, 128], mybir.dt.int32)
    fr = P.sb("fr", [128, 128], F32)
    fi = P.sb("fi", [128, 128], F32)
    PWr = P.sb("PWr", [128, 16, 128], F32)
    PWi = P.sb("PWi", [128, 16, 128], F32)
    P.dma("sp", If32[:], identd, w=["If32"])
    P.dma("sp", Jf32[:], jswd, w=["Jf32"])
    P.dma("sp", cv[:], cvec, w=["cv"])
    P.dma("sp", dcol[:], dcol_d, w=["dcol"])
    P.dma("sp", lr[:], ssm_lr, w=["lr"])
    P.dma("sp", li[:], ssm_li, w=["li"])
    P.dma("sp", ldt[:], ssm_ldt, w=["ldt"])
    T = lambda i: tmp[i][:]
    K = lambda i: ("tmp", i)
    dt_, rho, th, mag = 0, 1, 2, 3
    P.act(T(dt_), ldt[:], AF.Exp, r=["ldt"], w=[K(dt_)])
    P.tt("dve", T(rho), lr[:], T(dt_), ALU.mult, r=["lr", K(dt_)], w=[K(rho)])
    P.tt("dve", T(th), li[:], T(dt_), ALU.mult, r=["li", K(dt_)], w=[K(th)])
    P.act(T(mag), T(rho), AF.Exp, r=[K(rho)], w=[K(mag)])
    P.ts("dve", T(4), T(th), float(1.0 / (2 * np.pi)), None, ALU.mult, None, r=[K(th)], w=[K(4)])
    P.cp("dve", qi[:], T(4), r=[K(4)], w=["qi"])
    P.cp("dve", T(4), qi[:], r=["qi"], w=[K(4)])
    P.stt("dve", T(5), T(4), float(-2 * np.pi), T(th), ALU.mult, ALU.add, r=[K(4), K(th)], w=[K(5)])
    P.act(T(6), T(5), AF.Sin, r=[K(5)], w=[K(6)], scale=0.25)
    P.act(T(7), T(5), AF.Sin, r=[K(5), "cv"], w=[K(7)], scale=0.25, bias=cv[:, 3:4])
    for _ in range(2):
        P.stt("dve", T(4), T(6), 2.0, T(7), ALU.mult, ALU.mult, r=[K(6), K(7)], w=[K(4)])
        P.tt("dve", T(5), T(7), T(7), ALU.mult, r=[K(7)], w=[K(5)])
        P.tt("dve", T(0), T(6), T(6), ALU.mult, r=[K(6)], w=[K(0)])
        P.tt("dve", T(7), T(5), T(0), ALU.subtract, r=[K(5), K(0)], w=[K(7)])
        P.cp("dve", T(6), T(4), r=[K(4)], w=[K(6)])
    P.tt("dve", PWr[:, 0, :], T(mag), T(7), ALU.mult, r=[K(mag), K(7)], w=[("PW", 0)])
    P.tt("dve", PWi[:, 0, :], T(mag), T(6), ALU.mult, r=[K(mag), K(6)], w=[("PW", 0)])
    P.ts("dve", T(0), PWr[:, 0, :], -1.0, None, ALU.add, None, r=[("PW", 0)], w=[K(0)])
    P.tt("dve", T(1), lr[:], lr[:], ALU.mult, r=["lr"], w=[K(1)])
    P.tt("dve", T(2), li[:], li[:], ALU.mult, r=["li"], w=[K(2)])
    P.tt("dve", T(1), T(1), T(2), ALU.add, r=[K(1), K(2)], w=[K(1)])
    P.add("dve", lambda e: e.reciprocal(out=tmp[1][:], in_=tmp[1][:]), r=[K(1)], w=[K(1)])
    P.tt("dve", T(2), T(0), lr[:], ALU.mult, r=[K(0), "lr"], w=[K(2)])
    P.tt("dve", T(3), PWi[:, 0, :], li[:], ALU.mult, r=[("PW", 0), "li"], w=[K(3)])
    P.tt("dve", T(2), T(2), T(3), ALU.add, r=[K(2), K(3)], w=[K(2)])
    P.tt("dve", fr[:], T(2), T(1), ALU.mult, r=[K(2), K(1)], w=["fr"])
    P.tt("dve", T(2), PWi[:, 0, :], lr[:], ALU.mult, r=[("PW", 0), "lr"], w=[K(2)])
    P.tt("dve", T(3), T(0), li[:], ALU.mult, r=[K(0), "li"], w=[K(3)])
    P.tt("dve", T(2), T(2), T(3), ALU.subtract, r=[K(2), K(3)], w=[K(2)])
    P.tt("dve", fi[:], T(2), T(1), ALU.mult, r=[K(2), K(1)], w=["fi"])

    def cmul(o, a, b):
        P.tt("dve", T(0), PWr[:, a, :], PWr[:, b, :], ALU.mult, r=[("PW", a), ("PW", b)], w=[K(0)])
        P.tt("dve", T(1), PWi[:, a, :], PWi[:, b, :], ALU.mult, r=[("PW", a), ("PW", b)], w=[K(1)])
        P.tt("dve", T(2), PWr[:, a, :], PWi[:, b, :], ALU.mult, r=[("PW", a), ("PW", b)], w=[K(2)])
        P.tt("dve", T(3), PWi[:, a, :], PWr[:, b, :], ALU.mult, r=[("PW", a), ("PW", b)], w=[K(3)])
        P.tt("dve", PWr[:, o, :], T(0), T(1), ALU.subtract, r=[K(0), K(1)], w=[("PW", o)])
        P.tt("dve", PWi[:, o, :], T(2), T(3), ALU.add, r=[K(2), K(3)], w=[("PW", o)])

    for o in range(1, 16):
        sg = SIG[o]
        done = False
        for a in range(o - 1, -1, -1):
            for b in range(a, -1, -1):
                if SIG[a] + SIG[b] == sg and not done:
                    cmul(o, a, b)
                    done = True
        assert done
    P.ts("dve", PWi[:].rearrange("p a b -> p (a b)"), PWi[:].rearrange("p a b -> p (a b)"), cv[:, 0:1], None, ALU.mult, None,
         r=["PW", "cv"], w=["PW"])

    uT = [P.sb("uT%d" % i, [128, L], BF16) for i in range(2)]
    Crt = P.sb("Crt", [128, 16, 128], F32)
    Cit = P.sb("Cit", [128, 16, 128], F32)
    ct1 = P.sb("ct1", [128, 16, 128], F32)
    ct2 = P.sb("ct2", [128, 16, 128], F32)
    Cfin = P.sb("Cfin", [128, 16, 128], BF16)
    Bt = [P.sb("Bt%d" % i, [128, 8, 128], BF16) for i in range(2)]
    Rm = [P.sb("Rm%d" % i, [128, 16, 128], BF16) for i in range(2)]
    Rt = [P.sb("Rt%d" % i, [128, 128], F32) for i in range(2)]
    Xb = [P.sb("Xb%d" % i, [128, L], BF16) for i in range(2)]
    ysb = P.sb("ysb", [128, L], F32)
    yt1 = P.sb("yt1", [128, L], F32)
    px = [P.ps("px%d" % i, [128, 512], F32) for i in range(4)]
    py = [P.ps("py%d" % i, [128, 512], F32) for i in range(4)]
    ev = [0]

    def evac(dst, src, r, w):
        ev[0] += 1
        P.cp("act" if ev[0] % 2 else "dve", dst, src, r=r, w=w)

    rc = [0]
    bc = [0]
    for j in range(8):
        ub = j % 2
        P.dma("sp", uT[ub][:], fm_d[j], w=[("uT", ub)])
        P.dma("sp", Crt[:], Crp[j], w=["Crt"])
        P.dma("sp", Cit[:], Cip[j], w=["Cit"])
        frv = fr[:].rearrange("p (d g) -> p d g", d=2)[:, :, 8 * j:8 * j + 8].unsqueeze(3).to_broadcast([128, 2, 8, 128])
        fiv = fi[:].rearrange("p (d g) -> p d g", d=2)[:, :, 8 * j:8 * j + 8].unsqueeze(3).to_broadcast([128, 2, 8, 128])
        v4 = lambda t: t[:].rearrange("p (d g) c -> p d g c", d=2)
        P.tt("dve", v4(ct1), v4(Crt), frv, ALU.mult, r=["Crt", "fr"], w=["ct1"])
        P.tt("dve", v4(ct2), v4(Cit), fiv, ALU.mult, r=["Cit", "fi"], w=["ct2"])
        P.tt("dve", ct1[:], ct1[:], ct2[:], ALU.subtract, r=["ct1", "ct2"], w=["ct1"])
        P.tt("dve", v4(ct2), v4(Crt), fiv, ALU.mult, r=["Crt", "fi"], w=["ct2"])
        P.tt("dve", v4(Crt), v4(Cit), frv, ALU.mult, r=["Cit", "fr"], w=["Crt"])
        P.tt("dve", ct2[:], ct2[:], Crt[:], ALU.add, r=["ct2", "Crt"], w=["ct2"])
        P.ts("dve", ct2[:], ct2[:], cv[:, 2:3], None, ALU.mult, None, r=["ct2", "cv"], w=["ct2"])
        P.stt("dve", Cfin[:], ct1[:], cv[:, 1:2], ct2[:], ALU.mult, ALU.add, r=["ct1", "ct2", "cv"], w=["Cfin"])
        first_y = True
        for d in range(2):
            bb = bc[0] % 2
            bc[0] += 1
            P.dma("pool", Bt[bb][:], Bp[j, d], w=[("Bt", bb)])
            for g in range(8):
                dg = d * 64 + 8 * j + g
                rb = rc[0] % 2
                rc[0] += 1
                for si in range(16):
                    P.act(Rt[si % 2][:], Jf32[:], AF.Copy, r=["Jf32", "PW"], w=[("Rt", si % 2)], scale=PWi[:, si, dg:dg + 1])
                    P.stt("dve", Rm[rb][:, si, :], If32[:], PWr[:, si, dg:dg + 1], Rt[si % 2][:], ALU.mult, ALU.add,
                          r=["If32", "PW", ("Rt", si % 2)], w=[("Rm", rb)])
                xin, xout = 0, 1
                for b in range(4):
                    P.mm(px[b][:, :], lhsT=Bt[bb][:, g, :], rhs=uT[ub][:, 512 * b:512 * b + 512], start=True, stop=True,
                         r=[("Bt", bb), ("uT", ub)], w=[("px", b)])
                    evac(Xb[xin][:, 512 * b:512 * b + 512], px[b][:, :], r=[("px", b)], w=[("X", xin, b)])
                for ri, s_ in enumerate([1, 4, 16, 64, 256, 1024]):
                    for b in range(4):
                        lo, hi = 512 * b, 512 * b + 512
                        terms = []
                        for m in range(1, 4):
                            sg = m * s_
                            if sg >= L:
                                continue
                            si = SIG.index(sg)
                            if d == 0:
                                a0, a1 = max(lo, sg), hi
                                src0 = a0 - sg
                            else:
                                a0, a1 = lo, min(hi, L - sg)
                                src0 = a0 + sg
                            if a1 > a0:
                                terms.append((si, a0, a1, src0))
                        P.mm(px[b][:, :], lhsT=identb[:], rhs=Xb[xin][:, lo:hi], start=True, stop=(len(terms) == 0),
                             r=["identb", ("X", xin)], w=[("px", b)])
                        for ti, (si, a0, a1, src0) in enumerate(terms):
                            P.mm(px[b][:, a0 - lo:a1 - lo], lhsT=Rm[rb][:, si, :], rhs=Xb[xin][:, src0:src0 + (a1 - a0)],
                                 start=False, stop=(ti == len(terms) - 1), r=[("Rm", rb), ("X", xin)], w=[("px", b)])
                        evac(Xb[xout][:, lo:hi], px[b][:, :], r=[("px", b)], w=[("X", xout, b)])
                    xin, xout = xout, xin
                last_y = (d == 1 and g == 7)
                for b in range(4):
                    P.mm(py[b][:, :], lhsT=Cfin[:, d * 8 + g, :], rhs=Xb[xin][:, 512 * b:512 * b + 512], start=first_y, stop=last_y,
                         r=["Cfin", ("X", xin)], w=[("py", b)])
                first_y = False
        for b in range(4):
            P.stt("dve", ysb[:, 512 * b:512 * b + 512], uT[ub][:, 512 * b:512 * b + 512], dcol[:, j:j + 1], py[b][:, :],
                  ALU.mult, ALU.add, r=[("uT", ub), "dcol", ("py", b)], w=[("ysb", b)])
        if dbg == "p2":
            dbg_dump("y%d" % j, ysb[:], [128, L], F32, ["ysb"])
        P.act(yt1[:], ysb[:], AF.Square, r=["ysb"], w=["yt1"])
        P.ts("dve", yt1[:], yt1[:], 0.044715, 1.0, ALU.mult, ALU.add, r=["yt1"], w=["yt1"])
        P.tt("dve", yt1[:], yt1[:], ysb[:], ALU.mult, r=["yt1", "ysb"], w=["yt1"])
        P.act(yt1[:], yt1[:], AF.Sigmoid, r=["yt1"], w=["yt1"], scale=1.5957691216057308)
        P.tt("dve", zT[:, j, :], yt1[:], ysb[:], ALU.mult, r=["yt1", "ysb"], w=[("zT", j)])
    if dbg == "p2":
        dbg_dump("zT", zT[:], [128, 8, L], BF16, ["zT"])
        dbg_dump("PWr", PWr[:], [128, 16, 128], F32, ["PW"])
        dbg_dump("fr", fr[:], [128, 128], F32, ["fr"])
        P.end()
        carry.close()
        P.begin()
        return P.end(final=True)
    P.end()


    mixS = csb("mixS", [128, 8, L], BF16)
    P.begin()
    wglu = P.sb("wglu", [128, 8, 1024], BF16)
    sgt = [P.sb("sgt%d" % i, [128, 512], F32) for i in range(2)]
    zs = [P.sb("zs%d" % i, [128, 512], BF16) for i in range(2)]
    sq = [P.sb("sq%d" % i, [128, 512], BF16) for i in range(2)]
    ssr = P.sb("ssr", [128, 16], F32)
    pg = [P.ps("pg%d" % i, [128, 512], F32) for i in range(4)]
    pss = P.ps("pss", [128, 512], F32)
    P.dma("pool", wglu[:], w_glu.rearrange("(kt p) n -> p kt n", p=128), w=["wglu"])
    P.dma("sp", colv_t[:], colv, w=["colv"])
    P.memset("dve", onesc[:], 1.0, w=["onesc"])
    it = 0
    for tc in range(4):
        for mt in range(8):
            pi = it % 4
            bi = it % 2
            it += 1
            for kt in range(8):
                P.mm(pg[pi][:, :], lhsT=wglu[:, kt, 128 * mt:128 * mt + 128], rhs=zT[:, kt, 512 * tc:512 * tc + 512],
                     start=(kt == 0), stop=(kt == 7), r=["wglu", "zT"], w=[("pg", pi)])
            P.act(sgt[bi][:], pg[pi][:, :], AF.Sigmoid, r=[("pg", pi), "colv"], w=[("sgt", bi)], bias=colv_t[:, 0, mt:mt + 1])
            P.tt("dve", zs[bi][:], zT[:, mt, 512 * tc:512 * tc + 512], sgt[bi][:], ALU.mult, r=["zT", ("sgt", bi)], w=[("zs", bi)])
            P.act(sq[bi][:], zs[bi][:], AF.Square, r=[("zs", bi)], w=[("sq", bi)])
            P.ts("dve", mixS[:, mt, 512 * tc:512 * tc + 512], zs[bi][:], colv_t[:, 1, mt:mt + 1], None, ALU.mult, None,
                 r=[("zs", bi), "colv"], w=[("mixS", mt, tc)])
            for q in range(4):
                col = (4 * tc + q) * 8 + mt
                P.mm(pss[:, col:col + 1], lhsT=sq[bi][:, 128 * q:128 * q + 128], rhs=onesc[:, 0:1],
                     start=True, stop=True, r=[("sq", bi), "onesc"], w=["pss"])
    P.add("dve", lambda e: e.tensor_reduce(out=ssr[:], in_=pss[:, 0:128].rearrange("p (a m) -> p a m", m=8),
                                           axis=mybir.AxisListType.X, op=ALU.add), r=["pss"], w=["ssr"])
    P.ts("dve", ssr[:], ssr[:], 1.0 / 1024, EPS, ALU.mult, ALU.add, r=["ssr"], w=["ssr"])
    P.act(ssr[:], ssr[:], AF.Sqrt, r=["ssr"], w=["ssr"])
    P.add("dve", lambda e: e.reciprocal(out=rstd[:, 0, :], in_=ssr[:]), r=["ssr"], w=["rstd"])
    if dbg == "p3":
        dbg_dump("mixS", mixS[:], [128, 8, L], BF16, ["mixS"])
        dbg_dump("rstd", rstd[:], [128, 2, 16], F32, ["rstd"])
        P.end()
        carry.close()
        P.begin()
        return P.end(final=True)
    P.end()

    mixA = csb("mixA", [128, 8, L], BF16)
    P.begin()
    QT = [P.sb("QT%d" % i, [128, L], BF16) for i in range(2)]
    QTm = [P.sb("QTm%d" % i, [128, L], BF16) for i in range(2)]
    hmask = P.sb("hmask", [128, 2], F32)
    KT = [P.sb("KT%d" % i, [128, L], BF16) for i in range(2)]
    Vp = [P.sb("Vp%d" % i, [128, 31, 128], BF16) for i in range(2)]
    onesP = P.sb("onesP", [128, 2, 128], BF16)
    amask = P.sb("amask", [128, 64], F32)
    ebf = P.sb("ebf", [128, 2, 14, 64], F32)
    EBb = P.sb("EBb", [128, 2, 14, 64], BF16)
    Eb = [P.sb("Eb%d" % i, [128, 2, 4, 64], BF16) for i in range(2)]
    Pm = [P.sb("Pm%d" % i, [128, 2, 4, 64], BF16) for i in range(2)]
    rB = [P.sb("rB%d" % i, [128, 64], F32) for i in range(2)]
    sqa = P.sb("sqa", [128, 8, 128], BF16)
    ssr4 = P.sb("ssr4", [128, 16], F32)
    ps_s = [P.ps("ps_s%d" % i, [128, 2, 4, 64], F32) for i in range(2)]
    ps_a = [P.ps("ps_a%d" % i, [128, 512], F32) for i in range(2)]
    ps_b = [P.ps("ps_b%d" % i, [128, 512], F32) for i in range(2)]
    pss4 = P.ps("pss4", [128, 512], F32)
    P.dma("pool", onesP[:], onesP_d, w=["onesP"])
    P.dma("sp", amask[:], amask_d, w=["amask"])
    P.dma("sp", hmask[:], hmask_d, w=["hmask"])
    for hh in range(2):
        P.memset("dve", Vp[hh][:], 0.0, w=[("Vp", hh)])
    v_even = v_d.rearrange("(t p) c -> p t c", p=128)
    v_odd = v_d[64:64 + 1920, :].rearrange("(t p) c -> p t c", p=128)
    it = 0
    for pr in range(8):
        qb = pr % 2
        P.dma("sp", QT[qb][:], fm_d[8 + pr], w=[("QT", qb)])
        P.dma("sp", KT[qb][:], fm_d[16 + pr], w=[("KT", qb)])
        for hh in range(2):
            P.ts("dve", QTm[hh][:], QT[qb][:], hmask[:, hh:hh + 1], None, ALU.mult, None, r=[("QT", qb), "hmask"], w=[("QTm", hh)])
        for hh in range(2):
            h = 2 * pr + hh
            P.dma("sp", Vp[hh][:, 0:16, 64 * hh:64 * hh + 64], v_even[:, :, 64 * h:64 * h + 64], w=[("Vp", hh)])
            P.dma("sp", Vp[hh][:, 16:31, 64 * hh:64 * hh + 64], v_odd[:, :, 64 * h:64 * h + 64], w=[("Vp", hh)])
        P.dma("sp", ebf[:], biasE[pr], w=["ebf"])
        P.act(ebf[:], ebf[:], AF.Exp, r=["ebf"], w=["ebf"])
        P.tt("dve", EBb[:].rearrange("p a b c -> p (a b) c"), ebf[:].rearrange("p a b c -> p (a b) c"),
             amask[:].unsqueeze(1).to_broadcast([128, 28, 64]), ALU.mult, r=["ebf", "amask"], w=["EBb"])
        for r_ in range(32):
            rs = min(max(r_ - 4, 0), 24)
            dr0 = rs - r_ + 7
            bi = it % 2
            it += 1
            for hh in range(2):
                for m in range(4):
                    rho_ = rs + 2 * m
                    P.mm(ps_s[bi][:, hh, m, :], lhsT=KT[qb][:, 64 * rho_:64 * rho_ + 128],
                         rhs=QTm[hh][:, 64 * r_:64 * r_ + 64], start=True, stop=True,
                         r=[("KT", qb), ("QTm", hh)], w=[("ps_s", bi)])
            P.act(Eb[bi][:], ps_s[bi][:], AF.Exp, r=[("ps_s", bi)], w=[("Eb", bi)], scale=0.125)
            P.tt("dve", Pm[bi][:], Eb[bi][:], EBb[:, :, dr0:dr0 + 7:2, :], ALU.mult, r=[("Eb", bi), "EBb"], w=[("Pm", bi)])
            k = 0
            for hh in range(2):
                for m in range(4):
                    rho_ = rs + 2 * m
                    tix = rho_ // 2 if rho_ % 2 == 0 else 16 + (rho_ - 1) // 2
                    P.mm(ps_a[bi][:, 0:64], lhsT=Vp[hh][:, tix, :], rhs=Pm[bi][:, hh, m, :], start=(k == 0), stop=(k == 7),
                         r=[("Vp", hh), ("Pm", bi)], w=[("ps_a", bi)])
                    P.mm(ps_b[bi][:, 0:64], lhsT=onesP[:, hh, :], rhs=Pm[bi][:, hh, m, :], start=(k == 0), stop=(k == 7),
                         r=["onesP", ("Pm", bi)], w=[("ps_b", bi)])
                    k += 1
            P.add("dve", lambda e, o=rB[bi], i_=ps_b[bi]: e.reciprocal(out=o[:], in_=i_[:, 0:64]), r=[("ps_b", bi)], w=[("rB", bi)])
            P.tt("dve", mixA[:, pr, 64 * r_:64 * r_ + 64], ps_a[bi][:, 0:64], rB[bi][:], ALU.mult,
                 r=[("ps_a", bi), ("rB", bi)], w=[("mixA", pr, r_)])
    for tt_ in range(16):
        P.act(sqa[:], mixA[:, :, 128 * tt_:128 * tt_ + 128], AF.Square, r=["mixA"], w=["sqa"])
        for pr in range(8):
            P.mm(pss4[:, tt_:tt_ + 1], lhsT=sqa[:, pr, :], rhs=onesc[:, 0:1], start=(pr == 0), stop=(pr == 7),
                 r=["sqa", "onesc"], w=["pss4"])
    P.ts("dve", ssr4[:], pss4[:, 0:16], 1.0 / 1024, EPS, ALU.mult, ALU.add, r=["pss4"], w=["ssr4"])
    P.act(ssr4[:], ssr4[:], AF.Sqrt, r=["ssr4"], w=["ssr4"])
    P.add("dve", lambda e: e.reciprocal(out=rstd[:, 1, :], in_=ssr4[:]), r=["ssr4"], w=["rstd"])
    for pr in range(8):
        P.ts("dve", mixA[:, pr, :], mixA[:, pr, :], colv_t[:, 2, pr:pr + 1], None, ALU.mult, None, r=["mixA", "colv"], w=[("mixA", pr)])
    if dbg == "p4":
        dbg_dump("mixS", mixS[:], [128, 8, L], BF16, ["mixS"])
        dbg_dump("mixA", mixA[:], [128, 8, L], BF16, ["mixA"])
        dbg_dump("rstd", rstd[:], [128, 2, 16], F32, ["rstd"])
        P.end()
        carry.close()
        P.begin()
        return P.end(final=True)
    P.end()


    P.begin()
    wob = [P.sb("wob%d" % i, [128, 16, 512], BF16) for i in range(1)]
    xin = [P.sb("xin%d" % i, [128, 512], F32) for i in range(2)]
    t5 = [P.sb("t5_%d" % i, [128, 512], F32) for i in range(2)]
    xm = P.sb("xm", [128, D], F32)
    junk5 = P.sb("junk5", [128, 512], BF16)
    ss5 = P.sb("ss5", [128, 4], F32)
    sq5 = P.sb("sq5", [128, 4], F32)
    h2f = P.sb("h2f", [128, 16, 128], F32)
    h2t = [P.sb("h2t%d" % i, [128, 16, 128], BF16) for i in range(2)]
    h2lo = P.sb("h2lo", [128, 16, 128], BF16)
    xhi = P.sb("xhi", [128, D], BF16)
    xlo = P.sb("xlo", [128, D], BF16)
    whi = P.sb("whi", [128, 16, 16], BF16)
    wlo = P.sb("wlo", [128, 16, 16], BF16)
    gffn_t = P.sb("gffn_t", [128, 16], F32)
    G2 = P.sb("G2", [128, 16], F32)
    wr = P.sb("wr", [128, 16, 16], F32)
    lg = P.sb("lg", [128, 16], F32)
    sm = P.sb("sm", [128, 4], F32)
    pS = [P.ps("pS%d" % i, [128, 512], F32) for i in range(2)]
    pA = [P.ps("pA%d" % i, [128, 512], F32) for i in range(2)]
    pT = [P.ps("pT%d" % i, [128, 4, 128], F32) for i in range(2)]
    pL = P.ps("pL", [128, 512], F32)
    P.dma("sp", gffn_t[:], gffnT, w=["gffn_t"])
    P.dma("sp", wr[:], wr_d, w=["wr"])
    P.dma("sp", sel[:], sel_d, w=["sel"])
    P.dma("sp", If32p[:], identd, w=["If32p"])
    P.stt("dve", G2[:], modT[:, 64:80], 1.0, gffn_t[:], ALU.add, ALU.mult, r=["modT", "gffn_t"], w=["G2"])
    P.cp("dve", whi[:], wr[:], r=["wr"], w=["whi"])
    P.tt("dve", wlo[:], wr[:], whi[:], ALU.subtract, r=["wr", "whi"], w=["wlo"])
    w_out_v = w_out.rearrange("(kt p) n -> p kt n", p=128)
    it = 0
    for n in range(4):
        wi = 0
        P.dma("pool", wob[wi][:], w_out_v[:, :, 512 * n:512 * n + 512], w=[("wob", wi)])
        for tt_ in range(16):
            bi = it % 2
            it += 1
            P.dma("sp", xin[bi][:], x[128 * tt_:128 * tt_ + 128, 512 * n:512 * n + 512], w=[("xin", bi)])
            for kt in range(8):
                P.mm(pS[bi][:, :], lhsT=mixS[:, kt, 128 * tt_:128 * tt_ + 128], rhs=wob[wi][:, kt, :], start=(kt == 0), stop=(kt == 7),
                     r=["mixS", ("wob", wi)], w=[("pS", bi)])
            for kt in range(8):
                P.mm(pA[bi][:, :], lhsT=mixA[:, kt, 128 * tt_:128 * tt_ + 128], rhs=wob[wi][:, 8 + kt, :], start=(kt == 0), stop=(kt == 7),
                     r=["mixA", ("wob", wi)], w=[("pA", bi)])
            P.ts("dve", t5[bi][:], pS[bi][:, :], rstd[:, 0, tt_:tt_ + 1], None, ALU.mult, None, r=[("pS", bi), "rstd"], w=[("t5", bi)])
            P.stt("dve", t5[bi][:], pA[bi][:, :], rstd[:, 1, tt_:tt_ + 1], t5[bi][:], ALU.mult, ALU.add,
                  r=[("pA", bi), "rstd", ("t5", bi)], w=[("t5", bi)])
            P.tt("dve", t5[bi][:], t5[bi][:], gt1b[:, 512 * n:512 * n + 512], ALU.mult, r=[("t5", bi), "gtb"], w=[("t5", bi)])
            P.tt("dve", t5[bi][:], t5[bi][:], xin[bi][:], ALU.add, r=[("t5", bi), ("xin", bi)], w=[("t5", bi)])
            P.dma("sp", xmid_d[128 * tt_:128 * tt_ + 128, 512 * n:512 * n + 512], t5[bi][:], r=[("t5", bi)], w=[("xmid_d", tt_, n)])
    p5dump = '''
        P.end()
        carry.close()
        P.begin()
        t0 = P.sb("t0", [128, D], F32)
        for i in range(16):
            P.dma("sp", t0[:], xmid_d[128 * i:128 * i + 128, :], w=["t0"])
            o = nc.dram_tensor("dbg_xm%d" % i, [128, D], F32, kind="ExternalOutput").ap()
            P.out_dmas.append(P.dma("sp", o, t0[:], r=["t0"]))
    '''
    if dbg == "p5a":
        P.end()
        carry.close()
        P.begin()
        t0 = P.sb("t0", [128, D], F32)
        for i in range(16):
            P.dma("sp", t0[:], xmid_d[128 * i:128 * i + 128, :], w=["t0"])
            o = nc.dram_tensor("dbg_xm%d" % i, [128, D], F32, kind="ExternalOutput").ap()
            P.out_dmas.append(P.dma("sp", o, t0[:], r=["t0"]))
        return P.end(final=True)
    stage = int(dbg[3:]) if (dbg or "").startswith("p5s") else 99
    for tt_ in range(16 if stage == 99 else 1):
        P.dma("sp", xm[:], xmid_d[128 * tt_:128 * tt_ + 128, :], r=[("xmid_d", tt_)], w=["xm"])
        for c4 in range(4):
            P.act(junk5[:], xm[:, 512 * c4:512 * c4 + 512], AF.Square, r=["xm"], w=["junk5", "sq5"], accum_out=sq5[:, c4:c4 + 1])
        P.add("dve", lambda e: e.tensor_reduce(out=ss5[:, 0:1], in_=sq5[:], axis=mybir.AxisListType.X, op=ALU.add), r=["sq5"], w=["ss5"])
        P.ts("dve", ss5[:, 1:2], ss5[:, 0:1], 1.0 / D, EPS, ALU.mult, ALU.add, r=["ss5"], w=["ss5"])
        P.act(ss5[:, 2:3], ss5[:, 1:2], AF.Sqrt, r=["ss5"], w=["ss5"])
        P.add("dve", lambda e: e.reciprocal(out=ss5[:, 3:4], in_=ss5[:, 2:3]), r=["ss5"], w=["ss5"])
        P.ts("dve", xm[:], xm[:], ss5[:, 3:4], None, ALU.mult, None, r=["ss5", "xm"], w=["xm"])
        if stage < 1:
            break
        P.cp("dve", xhi[:], xm[:], r=["xm"], w=["xhi"])
        P.tt("dve", xlo[:], xm[:], xhi[:], ALU.subtract, r=["xm", "xhi"], w=["xlo"])
        for q4 in range(4):
            pi = q4 % 2
            for q in range(4):
                kt = 4 * q4 + q
                P.mm(pT[pi][:, q, :], lhsT=xhi[:, 128 * kt:128 * kt + 128], rhs=identb[:], start=True, stop=False, r=["xhi", "identb"], w=[("pT", pi)])
                P.mm(pT[pi][:, q, :], lhsT=xlo[:, 128 * kt:128 * kt + 128], rhs=identb[:], start=False, stop=True, r=["xlo", "identb"], w=[("pT", pi)])
            for q in range(4):
                kt = 4 * q4 + q
                if False:
                    P.act(h2f[:, kt, :], pT[pi][:, q, :], AF.Identity, r=[("pT", pi), "G2", "modT"], w=[("h2f", kt)],
                          scale=G2[:, kt:kt + 1], bias=modT[:, 48 + kt:48 + kt + 1])
                else:
                    P.ts("dve", h2f[:, kt, :], pT[pi][:, q, :], G2[:, kt:kt + 1], modT[:, 48 + kt:48 + kt + 1], ALU.mult, ALU.add,
                         r=[("pT", pi), "G2", "modT"], w=[("h2f", kt)])
        if stage < 2:
            break
        hb = tt_ % 2
        P.cp("dve", h2t[hb][:], h2f[:], r=["h2f"], w=[("h2t", hb)])
        P.tt("dve", h2lo[:], h2f[:], h2t[hb][:], ALU.subtract, r=["h2f", ("h2t", hb)], w=["h2lo"])
        for kt in range(16):
            P.mm(pL[:, 0:16], lhsT=h2t[hb][:, kt, :], rhs=whi[:, kt, :], start=(kt == 0), stop=False, r=[("h2t", hb), "whi"], w=["pL"])
            P.mm(pL[:, 0:16], lhsT=h2lo[:, kt, :], rhs=whi[:, kt, :], start=False, stop=False, r=["h2lo", "whi"], w=["pL"])
            P.mm(pL[:, 0:16], lhsT=h2t[hb][:, kt, :], rhs=wlo[:, kt, :], start=False, stop=(kt == 15), r=[("h2t", hb), "wlo"], w=["pL"])
        if stage < 3:
            break
        P.dma("sp", h2all_d[:, :, 128 * tt_:128 * tt_ + 128], h2t[hb][:], r=[("h2t", hb)], w=[("h2all_d", tt_)])
        if stage < 4:
            break
        P.cp("dve", lg[:], pL[:, 0:16], r=["pL"], w=["lg"])
        P.add("dve", lambda e: e.tensor_reduce(out=sm[:, 0:1], in_=lg[:], axis=mybir.AxisListType.X, op=ALU.max), r=["lg"], w=["sm"])
        P.ts("dve", sm[:, 1:2], sm[:, 0:1], -1.0, None, ALU.mult, None, r=["sm"], w=["sm"])
        P.act(lg[:], lg[:], AF.Exp, r=["lg", "sm"], w=["lg", "sm2"], bias=sm[:, 1:2], accum_out=sm[:, 2:3])
        P.add("dve", lambda e: e.reciprocal(out=sm[:, 3:4], in_=sm[:, 2:3]), r=["sm2", "sm"], w=["sm"])
        P.ts("dve", aff_all[:, tt_, :], lg[:], sm[:, 3:4], None, ALU.mult, None, r=["lg", "sm"], w=[("aff", tt_)])
    if (dbg or "").startswith("p5s"):
        dbg_dump("ss5", ss5[:], [128, 4], F32, ["ss5"])
        P.end()
        carry.close()
        P.begin()
        return P.end(final=True)
    if dbg == "p5":
        dbg_dump("aff", aff_all[:], [128, 16, 16], F32, ["aff"])
        P.end()
        carry.close()
        P.begin()
        t1_ = P.sb("t1_", [128, 16, L], BF16)
        P.dma("sp", t1_[:], h2all_d, w=["t1_"])
        dbg_dump("h2all", t1_[:], [128, 16, L], BF16, ["t1_"])
        t0 = P.sb("t0", [128, D], F32)
        for i in range(16):
            P.dma("sp", t0[:], xmid_d[128 * i:128 * i + 128, :], w=["t0"])
            o = nc.dram_tensor("dbg_xm%d" % i, [128, D], F32, kind="ExternalOutput").ap()
            P.out_dmas.append(P.dma("sp", o, t0[:], r=["t0"]))
        return P.end(final=True)
    P.end()
    carry.close()

    carry2 = contextlib.ExitStack()
    xacc = carry2.enter_context(nc.sbuf_tensor("xacc_c2", [128, 8, D], F32))
    sc_own = carry2.enter_context(nc.sbuf_tensor("sc_own_c2", [128, 8, 16], F32))
    P.begin()
    affT = P.sb("affT", [16, L], F32)
    work = P.sb("work", [16, L], F32)
    max8 = P.sb("max8", [16, 8], F32)
    sc_all = P.sb("sc_all", [128, 16, 16], F32)
    xld = [P.sb("xld%d" % i, [128, D], F32) for i in range(2)]
    pX = P.ps("pX", [128, 512], F32)
    ahi = P.sb("ahi", [128, 16, 16], BF16)
    alo = P.sb("alo", [128, 16, 16], BF16)
    khi = P.sb("khi", [16, L], BF16)
    klo = P.sb("klo", [16, L], BF16)
    P.cp("dve", ahi[:], aff_all[:], r=["aff"], w=["ahi"])
    P.tt("dve", alo[:], aff_all[:], ahi[:], ALU.subtract, r=["aff", "ahi"], w=["alo"])
    for q4 in range(4):
        for q in range(4):
            tt_ = 4 * q4 + q
            P.mm(pX[0:16, 128 * q:128 * q + 128], lhsT=ahi[:, tt_, :], rhs=identb[:], start=True, stop=False, r=["ahi", "identb"], w=["pX"])
            P.mm(pX[0:16, 128 * q:128 * q + 128], lhsT=alo[:, tt_, :], rhs=identb[:], start=False, stop=True, r=["alo", "identb"], w=["pX"])
        P.cp("dve", affT[:, 512 * q4:512 * q4 + 512], pX[0:16, :], r=["pX"], w=["affT"])
    P.cp("dve", work[:], affT[:], r=["affT"], w=["work"])
    for _ in range(32):
        P.add("dve", lambda e: e.max(out=max8[:], in_=work[:]), r=["work"], w=["max8"])
        P.add("dve", lambda e: e.match_replace(out=work[:], in_to_replace=max8[:], in_values=work[:], imm_value=0.0),
              r=["work", "max8"], w=["work"])
    P.tt("dve", work[:], affT[:], work[:], ALU.subtract, r=["affT", "work"], w=["work"])
    P.cp("dve", khi[:], work[:], r=["work"], w=["khi"])
    P.tt("dve", klo[:], work[:], khi[:], ALU.subtract, r=["work", "khi"], w=["klo"])
    for q4 in range(4):
        for q in range(4):
            tt_ = 4 * q4 + q
            P.mm(pX[:, 16 * q:16 * q + 16], lhsT=khi[:, 128 * tt_:128 * tt_ + 128], rhs=identb[0:16, 0:16], start=True, stop=False, r=["khi", "identb"], w=["pX"])
            P.mm(pX[:, 16 * q:16 * q + 16], lhsT=klo[:, 128 * tt_:128 * tt_ + 128], rhs=identb[0:16, 0:16], start=False, stop=True, r=["klo", "identb"], w=["pX"])
        P.cp("dve", sc_all[:, 4 * q4:4 * q4 + 4, :], pX[:, 0:64].rearrange("p (a b) -> p a b", b=16), r=["pX"], w=["sc_all"])
    P.ts("dve", sc_own[:], sc_all[:, 0:8, :], sel[:, 0:1], None, ALU.mult, None, r=["sc_all", "sel"], w=["sc_own"])
    P.stt("dve", sc_own[:], sc_all[:, 8:16, :], sel[:, 1:2], sc_own[:], ALU.mult, ALU.add, r=["sc_all", "sel", "sc_own"], w=["sc_own"])
    for t8 in range(8):
        P.dma("sp", xld[0][:], xmid_d[128 * t8:128 * t8 + 128, :], w=[("xld", 0)])
        P.dma("sp", xld[1][:], xmid_d[128 * (t8 + 8):128 * (t8 + 8) + 128, :], w=[("xld", 1)])
        P.ts("dve", xacc[:, t8, :], xld[0][:], sel[:, 0:1], None, ALU.mult, None, r=[("xld", 0), "sel"], w=[("xacc", t8)])
        P.stt("dve", xacc[:, t8, :], xld[1][:], sel[:, 1:2], xacc[:, t8, :], ALU.mult, ALU.add, r=[("xld", 1), "sel", ("xacc", t8)], w=[("xacc", t8)])
    if dbg == "p6a":
        dbg_dump("sc_own", sc_own[:], [128, 8, 16], F32, ["sc_own"])
        dbg_dump("sc_all", sc_all[:], [128, 16, 16], F32, ["sc_all"])
        P.end()
        carry2.close()
        P.begin()
        return P.end(final=True)
    P.end()

    P.begin()
    h2o = P.sb("h2o", [128, 16, 1024], BF16)
    hidT = P.sb("hidT", [128, 12, 1024], BF16)
    wg = [P.sb("wg%d" % i, [128, 16, 256], BF16) for i in range(2)]
    wu = [P.sb("wu%d" % i, [128, 16, 256], BF16) for i in range(2)]
    wd = [P.sb("wd%d" % i, [128, 12, 512], BF16) for i in range(1)]
    sgt6 = [P.sb("sgt6_%d" % i, [128, 512], F32) for i in range(2)]
    t6 = [P.sb("t6_%d" % i, [128, 512], F32) for i in range(2)]
    pG = [P.ps("pG%d" % i, [128, 512], F32) for i in range(2)]
    pU = [P.ps("pU%d" % i, [128, 512], F32) for i in range(2)]
    pD = [P.ps("pD%d" % i, [128, 512], F32) for i in range(2)]
    P.dma("sp", h2o[:], h2all_d[:, :, 0:1024], w=["h2o"])
    for kh in range(2):
        P.dma("sp", hidT[:, 0:8, :], h2all_d[:, 8 * kh:8 * kh + 8, 1024:2048], w=["hidT"])
        P.ts("dve", h2o[:, 8 * kh:8 * kh + 8, :], h2o[:, 8 * kh:8 * kh + 8, :], sel[:, 0:1], None, ALU.mult, None, r=["h2o", "sel"], w=["h2o"])
        P.stt("dve", h2o[:, 8 * kh:8 * kh + 8, :], hidT[:, 0:8, :], sel[:, 1:2], h2o[:, 8 * kh:8 * kh + 8, :], ALU.mult, ALU.add,
              r=["hidT", "sel", "h2o"], w=["h2o"])
    wc = 0
    it = 0
    NE = 16 if dbg != "p6s" else 1
    for e_ in range(NE):
        wgv = w_gate[e_].rearrange("(kt p) f -> p kt f", p=128)
        wuv = w_up[e_].rearrange("(kt p) f -> p kt f", p=128)
        wdv = w_down[e_].rearrange("(fi p) c -> p fi c", p=128)
        for half, (c0, c1) in enumerate([(0, 6), (6, 11)]):
            nft = 2 * (c1 - c0)
            for ch in range(c0, c1):
                wi = wc % 2
                wc += 1
                P.dma("pool", wg[wi][:], wgv[:, :, 256 * ch:256 * ch + 256], w=[("wg", wi)])
                P.dma("pool", wu[wi][:], wuv[:, :, 256 * ch:256 * ch + 256], w=[("wu", wi)])
                for sub in range(2):
                    fi = 2 * (ch - c0) + sub
                    for tc in range(2):
                        bi = it % 2
                        it += 1
                        for kt in range(16):
                            P.mm(pG[bi][:, :], lhsT=wg[wi][:, kt, 128 * sub:128 * sub + 128], rhs=h2o[:, kt, 512 * tc:512 * tc + 512],
                                 start=(kt == 0), stop=(kt == 15), r=[("wg", wi), "h2o"], w=[("pG", bi)])
                        for kt in range(16):
                            P.mm(pU[bi][:, :], lhsT=wu[wi][:, kt, 128 * sub:128 * sub + 128], rhs=h2o[:, kt, 512 * tc:512 * tc + 512],
                                 start=(kt == 0), stop=(kt == 15), r=[("wu", wi), "h2o"], w=[("pU", bi)])
                        P.act(sgt6[bi][:], pG[bi][:, :], AF.Silu, r=[("pG", bi)], w=[("sgt6", bi)])
                        P.tt("dve", hidT[:, fi, 512 * tc:512 * tc + 512], sgt6[bi][:], pU[bi][:, :], ALU.mult,
                             r=[("sgt6", bi), ("pU", bi)], w=[("hidT", fi)])
            for dc in range(4):
                di = 0
                P.dma("pool", wd[di][:, 0:nft, :], wdv[:, 2 * c0:2 * c0 + nft, 512 * dc:512 * dc + 512], w=[("wd", di)])
                for t8 in range(8):
                    bi = it % 2
                    it += 1
                    for fi in range(nft):
                        P.mm(pD[bi][:, :], lhsT=hidT[:, fi, 128 * t8:128 * t8 + 128], rhs=wd[di][:, fi, :], start=(fi == 0), stop=(fi == nft - 1),
                             r=["hidT", ("wd", di)], w=[("pD", bi)])
                    P.tt("dve", t6[bi][:], pD[bi][:, :], gt2b[:, 512 * dc:512 * dc + 512], ALU.mult, r=[("pD", bi), "gtb"], w=[("t6", bi)])
                    P.stt("dve", xacc[:, t8, 512 * dc:512 * dc + 512], t6[bi][:], sc_own[:, t8, e_:e_ + 1], xacc[:, t8, 512 * dc:512 * dc + 512],
                          ALU.mult, ALU.add, r=[("t6", bi), "sc_own", ("xacc", t8, dc)], w=[("xacc", t8, dc)])
    P.end()

    P.begin()
    gfin_t = P.sb("gfin_t", [128, D], F32)
    ss6 = P.sb("ss6", [128, 4], F32)
    junk6 = P.sb("junk6", [128, D], BF16)
    P.dma("sp", gfin_t[:], gfin, w=["gfin_t"])
    for t8 in range(8):
        P.act(junk6[:], xacc[:, t8, :], AF.Square, r=[("xacc", t8)], w=["junk6", "ss6"], accum_out=ss6[:, 0:1])
        P.ts("dve", ss6[:, 1:2], ss6[:, 0:1], 1.0 / D, EPS, ALU.mult, ALU.add, r=["ss6"], w=["ss6"])
        P.act(ss6[:, 2:3], ss6[:, 1:2], AF.Sqrt, r=["ss6"], w=["ss6"])
        P.add("dve", lambda e: e.reciprocal(out=ss6[:, 3:4], in_=ss6[:, 2:3]), r=["ss6"], w=["ss6"])
        P.stt("dve", xacc[:, t8, :], xacc[:, t8, :], ss6[:, 3:4], gfin_t[:], ALU.mult, ALU.mult, r=["ss6", ("xacc", t8), "gfin_t"], w=[("xacc", t8)])
        P.out_dmas.append(P.dma("sp", out[128 * t8:128 * t8 + 128, :], xacc[:, t8, :], r=[("xacc", t8)]))
    nc_ = P.end(final=True)
    carry2.close()
    P.pstack.close()
    return nc_


def make_inputs(inputs, core, moe=True):
    b = core // 2
    f = np.float32
    g = lambda k: np.asarray(inputs[k], dtype=f)
    m = {}
    m["x"] = np.ascontiguousarray(g("x")[b])
    m["cT"] = np.ascontiguousarray(g("c")[b].reshape(16, 128).T)
    m["w_ada"] = np.ascontiguousarray(g("w_ada")[0])
    m["badaT"] = np.ascontiguousarray(g("b_ada")[0].reshape(96, 128).T)
    m["bada"] = np.ascontiguousarray(g("b_ada")[0].reshape(1, -1))
    m["gmixT"] = np.ascontiguousarray(g("g_mix")[0].reshape(16, 128).T)
    m["w_in"] = np.ascontiguousarray(g("w_in")[0])
    m["identd"] = np.eye(128, dtype=f)
    m["gfin"] = np.ascontiguousarray(np.broadcast_to(g("g_final").reshape(1, -1), (128, D)))
    jsw = np.zeros((128, 128), f)
    jsw[np.arange(128), (np.arange(128) + 64) % 128] = 1.0
    m["jswd"] = jsw
    dup = lambda a: np.ascontiguousarray(np.concatenate([a, a], 0))
    m["ssm_lr"] = dup(g("ssm_a_re")[0].transpose(2, 0, 1).reshape(64, 128))
    m["ssm_li"] = dup(g("ssm_a_im")[0].transpose(2, 0, 1).reshape(64, 128))
    m["ssm_ldt"] = np.ascontiguousarray(np.broadcast_to(g("ssm_log_dt")[0].reshape(1, 128), (128, 128)))
    cv = np.zeros((128, 4), f)
    cv[:64, 0] = 1.0; cv[64:, 0] = -1.0
    cv[:64, 1] = 1.0
    cv[64:, 2] = -1.0
    cv[:, 3] = np.pi / 2
    m["cvec"] = cv
    bre, bim = g("ssm_b_re")[0], g("ssm_b_im")[0]
    Bp = np.zeros((8, 2, 128, 8, 128), f)
    Cr = np.zeros((8, 128, 16, 128), f)
    Ci = np.zeros((8, 128, 16, 128), f)
    cre, cim = g("ssm_c_re")[0], g("ssm_c_im")[0]
    for j in range(8):
        for d in range(2):
            for gg in range(8):
                G = 8 * j + gg
                Bp[j, d, 16 * gg:16 * gg + 16, gg, 0:64] = bre[d, G].T
                Bp[j, d, 16 * gg:16 * gg + 16, gg, 64:128] = bim[d, G].T
                Cr[j, 0:64, d * 8 + gg, 16 * gg:16 * gg + 16] = cre[d, G].T
                Cr[j, 64:128, d * 8 + gg, 16 * gg:16 * gg + 16] = cre[d, G].T
                Ci[j, 0:64, d * 8 + gg, 16 * gg:16 * gg + 16] = cim[d, G].T
                Ci[j, 64:128, d * 8 + gg, 16 * gg:16 * gg + 16] = cim[d, G].T
    m["Bp"], m["Crp"], m["Cip"] = Bp, Cr, Ci
    m["dcol"] = np.ascontiguousarray(g("ssm_d")[0].reshape(8, 128).T)
    m["w_glu"] = np.ascontiguousarray(g("w_glu")[0])
    m["colv"] = np.ascontiguousarray(np.stack([g("b_glu")[0].reshape(8, 128).T, g("g_ssm_out")[0].reshape(8, 128).T,
                                               g("g_attn_out")[0].reshape(8, 128).T], 1))
    rpb = g("rpb")[0]
    wk = np.arange(64)[:, None]
    wq = np.arange(64)[None, :]
    dc = np.clip(wk - wq + 15, 0, 30)
    bE = np.zeros((8, 2, 64, 2, 14, 64), f)
    for j in range(2):
        for dr0 in range(14):
            t = rpb[:, dr0 + j][:, dc]
            bE[:, j, :, :, dr0, :] = t.reshape(8, 2, 64, 64).transpose(0, 2, 1, 3)
    m["biasE"] = np.ascontiguousarray(bE.reshape(8, 128, 2, 14, 64))
    cs = np.clip(np.arange(64) - 8, 0, 48)[None, :]
    am = ((wk >= cs) & (wk < cs + 16)).astype(f)
    m["amask"] = np.ascontiguousarray(np.concatenate([am, am], 0))
    oP = np.zeros((128, 2, 128), f)
    oP[:, 0, 0:64] = 1.0
    oP[:, 1, 64:128] = 1.0
    m["onesP"] = oP
    hm = np.zeros((128, 2), f)
    hm[:64, 0] = 1.0
    hm[64:, 1] = 1.0
    m["hmask"] = hm
    m["w_out"] = np.ascontiguousarray(g("w_out")[0])
    m["gffnT"] = np.ascontiguousarray(g("g_ffn")[0].reshape(16, 128).T)
    m["wr"] = np.ascontiguousarray(g("w_router")[0].reshape(16, 128, 16).transpose(1, 0, 2))
    half = core % 2
    sl = np.zeros((128, 2), f)
    sl[:, half] = 1.0
    m["sel"] = sl
    if moe:
        m["w_gate"] = np.ascontiguousarray(g("w_gate")[0])
        m["w_up"] = np.ascontiguousarray(g("w_up")[0])
        m["w_down"] = np.ascontiguousarray(g("w_down")[0])
    return m


def kernel(**inputs):
    nc = build()
    in_maps = [make_inputs(inputs, c) for c in range(8)]
    res = run_bass_kernel_spmd(nc, in_maps, core_ids=list(range(8)))
    out = np.zeros((4, L, D), np.float32)
    for c in range(8):
        b, half = c // 2, c % 2
        out[b, half * 1024:(half + 1) * 1024] = np.asarray(res.results[c]["out"])
    return out
```
